# Optimizing a Trainium2 kernel written in Bass

```python
import jax, jax.numpy as jnp
from jax import lax
import numpy as np

D_MODEL = 4096
BATCH = 2
SEQ = 4096
DEPTH = 2

CTX_LEN = 256
GRID_W = 64
MIX_WIDTH = D_MODEL
N_MOD = 6
EPS = 1e-6
N_DIRS = 2
MLSTM_HEADS = 4
MLSTM_WIDTH = D_MODEL // 2
MLSTM_DV = MLSTM_WIDTH // MLSTM_HEADS
MLSTM_DK = MLSTM_DV // 2
MLSTM_CHUNK = 128
MLSTM_QK_WIDTH = MLSTM_HEADS * MLSTM_DK
MLSTM_GATE_COLS = N_DIRS * 2 * MLSTM_HEADS
GMLP_HEADS = 4
GMLP_WIDTH = D_MODEL // 4
GMLP_HEAD_DIM = GMLP_WIDTH // GMLP_HEADS
GMLP_CHUNK = 128
CONV_WIDTH = D_MODEL // 4
CONV_K = 3
N_EXPERTS = 16
N_EXPERT_GROUPS = 4
EXPERTS_PER_GROUP = N_EXPERTS // N_EXPERT_GROUPS
TOP_K = 2
D_FF_EXPERT = D_MODEL // 4
IN_SIZES = (MLSTM_QK_WIDTH, MLSTM_QK_WIDTH, MLSTM_WIDTH, MLSTM_WIDTH, MLSTM_GATE_COLS, GMLP_WIDTH, GMLP_WIDTH, CONV_WIDTH, CONV_WIDTH, CONV_WIDTH)
IN_COLS = sum(IN_SIZES)

kernel_name = 'hybrid_mlstm_gmlp_conv_moe_dit'


def rms_norm(x, g):
    xf = x.astype(jnp.float32)
    y = xf * lax.rsqrt(jnp.mean(xf * xf, axis=-1, keepdims=True) + EPS)
    return (y * g.astype(jnp.float32)).astype(x.dtype)


def modulate(h, shift, scale):
    return h * (1 + scale) + shift


def split_cols(z):
    outs, start = [], 0
    for n in IN_SIZES:
        outs.append(z[..., start:start + n])
        start += n
    return outs


def zero_mlstm_state(bsz):
    f32 = jnp.float32
    return (jnp.zeros((bsz, MLSTM_HEADS, MLSTM_DK, MLSTM_DV), f32),
            jnp.zeros((bsz, MLSTM_HEADS, MLSTM_DK), f32),
            jnp.zeros((bsz, MLSTM_HEADS), f32))


def mlstm_chunkwise(q, k, v, i_pre, f_pre, state):
    bsz, nh, t, _ = q.shape
    n_chunks = t // MLSTM_CHUNK

    def to_chunks(a):
        a = a.reshape((bsz, nh, n_chunks, MLSTM_CHUNK) + a.shape[3:])
        return jnp.moveaxis(a, 2, 0)

    log_f = jax.nn.log_sigmoid(f_pre)
    mask = jnp.tril(jnp.ones((MLSTM_CHUNK, MLSTM_CHUNK), dtype=bool))

    def step(carry, inp):
        c_prev, n_prev, m_prev = carry
        qc, kc, vc, ic, lfc = inp
        b = jnp.cumsum(lfc, axis=-1)
        d = jnp.where(mask, b[..., :, None] - b[..., None, :] + ic[..., None, :], -jnp.inf)
        m_inter = b + m_prev[..., None]
        m_t = jnp.maximum(jnp.max(d, axis=-1), m_inter)
        s = jnp.einsum('bhtd,bhsd->bhts', qc, kc) * jnp.exp(d - m_t[..., None])
        inter = jnp.exp(m_inter - m_t)
        num = jnp.einsum('bhts,bhsv->bhtv', s, vc) + inter[..., None] * jnp.einsum('bhtd,bhdv->bhtv', qc, c_prev)
        den = jnp.sum(s, axis=-1) + inter * jnp.einsum('bhtd,bhd->bht', qc, n_prev)
        h = num / jnp.maximum(jnp.abs(den), jnp.exp(-m_t))[..., None]
        b_last = b[..., -1]
        g = b_last[..., None] - b + ic
        m_new = jnp.maximum(b_last + m_prev, jnp.max(g, axis=-1))
        decay = jnp.exp(b_last + m_prev - m_new)
        wk = jnp.exp(g - m_new[..., None])
        c_new = decay[..., None, None] * c_prev + jnp.einsum('bhs,bhsd,bhsv->bhdv', wk, kc, vc)
        n_new = decay[..., None] * n_prev + jnp.einsum('bhs,bhsd->bhd', wk, kc)
        return (c_new, n_new, m_new), h

    final, h = lax.scan(step, state, (to_chunks(q), to_chunks(k), to_chunks(v), to_chunks(i_pre), to_chunks(log_f)))
    h = jnp.moveaxis(h, 0, 2).reshape(bsz, nh, t, -1)
    return h, final


def mlstm_mixer(q, k, v, o, gate_pre, b_gate, norm_g, init_states):
    bsz, t, _ = q.shape
    f32 = jnp.float32

    def heads(a, dh):
        return a.reshape(bsz, t, MLSTM_HEADS, dh).transpose(0, 2, 1, 3).astype(f32)

    qh = heads(q, MLSTM_DK) * (MLSTM_DK ** -0.5)
    kh = heads(k, MLSTM_DK)
    vh = heads(v, MLSTM_DV)
    g = gate_pre.astype(f32).reshape(bsz, t, N_DIRS, 2, MLSTM_HEADS) + b_gate.astype(f32)
    g = g.transpose(2, 3, 0, 4, 1)
    if init_states is None:
        init_states = (zero_mlstm_state(bsz), zero_mlstm_state(bsz))

    def flip(a):
        return jnp.flip(a, axis=2)

    h_f, s_f = mlstm_chunkwise(qh, kh, vh, g[0, 0], g[0, 1], init_states[0])
    h_b, s_b = mlstm_chunkwise(flip(qh), flip(kh), flip(vh), flip(g[1, 0]), flip(g[1, 1]), init_states[1])
    h = h_f + flip(h_b)
    h = h * lax.rsqrt(jnp.mean(h * h, axis=-1, keepdims=True) + EPS)
    h = h.transpose(0, 2, 1, 3).reshape(bsz, t, MLSTM_WIDTH) * norm_g.astype(f32)
    out = h * jax.nn.sigmoid(o.astype(f32))
    return out.astype(q.dtype), (s_f, s_b)


def gmlp_mixer(u, v, w_s, b_s, norm_g):
    bsz, t, _ = u.shape
    u = jax.nn.gelu(u)
    v = rms_norm(jax.nn.gelu(v), norm_g)
    vc = v.reshape(bsz, t // GMLP_CHUNK, GMLP_CHUNK, GMLP_HEADS, GMLP_HEAD_DIM)
    s = jnp.einsum('hts,bnshd->bnthd', w_s, vc) + b_s.T[None, None, :, :, None]
    return u * s.reshape(bsz, t, GMLP_WIDTH)


def centred_conv3(y, w):
    pad = [(0, 0)] * (y.ndim - 2) + [(1, 1), (0, 0)]
    yp = jnp.pad(y, pad)
    return w[0] * yp[..., :-2, :] + w[1] * yp[..., 1:-1, :] + w[2] * yp[..., 2:, :]


def conv_mixer(bg, cg, xin, w, rows):
    y = cg * xin
    if rows is None:
        y = centred_conv3(y, w)
    else:
        bsz, t, ch = y.shape
        y = centred_conv3(y.reshape(bsz, rows, GRID_W, ch), w).reshape(bsz, t, ch)
    return bg * y


def moe_ffn(h, w_router, b_router, w_gate, w_up, w_down):
    shp = h.shape
    hf = h.reshape(-1, shp[-1])
    n = hf.shape[0]
    f32 = jnp.float32
    scores = jax.nn.sigmoid(jnp.dot(hf.astype(f32), w_router.astype(f32)))
    sel = (scores + b_router.astype(f32)).reshape(n, N_EXPERT_GROUPS, EXPERTS_PER_GROUP)
    group_score = jnp.sum(lax.top_k(sel, TOP_K)[0], axis=-1)
    top_group = lax.top_k(group_score, 1)[1][:, 0]
    in_group = sel[jnp.arange(n), top_group]
    local = lax.top_k(in_group, TOP_K)[1]
    expert_idx = top_group[:, None] * EXPERTS_PER_GROUP + local
    w_sel = jnp.take_along_axis(scores, expert_idx, axis=1)
    w_sel = w_sel / jnp.sum(w_sel, axis=-1, keepdims=True)
    combine = jnp.sum(jax.nn.one_hot(expert_idx, N_EXPERTS, dtype=f32) * w_sel[..., None], axis=1)
    combine = combine.astype(h.dtype)
    out = jnp.zeros_like(hf)
    for e in range(N_EXPERTS):
        a = jax.nn.silu(hf @ w_gate[e]) * (hf @ w_up[e])
        out = out + combine[:, e:e + 1] * (a @ w_down[e])
    return out.reshape(shp)


def setup_inputs(seed: int = 0) -> dict:
    key = jax.random.key(seed)
    ks = jax.random.split(key, 24)
    f32 = jnp.float32

    def nrm(k, shape, scale):
        return jax.random.normal(k, shape, f32) * scale

    b_i = nrm(ks[10], (DEPTH, N_DIRS, 1, MLSTM_HEADS), 0.1)
    b_f = jnp.linspace(3.0, 6.0, MLSTM_HEADS, dtype=f32) + nrm(ks[11], (DEPTH, N_DIRS, 1, MLSTM_HEADS), 0.1)
    return {
        'x': nrm(ks[0], (BATCH, SEQ, D_MODEL), 1.0),
        'c': nrm(ks[1], (BATCH, D_MODEL), 1.0),
        'ctx': nrm(ks[2], (BATCH, CTX_LEN, D_MODEL), 1.0),
        'c_ctx': nrm(ks[3], (D_MODEL,), 1.0),
        'w_mod': nrm(ks[4], (DEPTH, D_MODEL, N_MOD * D_MODEL), 0.5 * D_MODEL ** -0.5),
        'b_mod': nrm(ks[5], (DEPTH, N_MOD * D_MODEL), 0.02),
        'norm1_g': 1.0 + nrm(ks[6], (DEPTH, D_MODEL), 0.02),
        'norm2_g': 1.0 + nrm(ks[7], (DEPTH, D_MODEL), 0.02),
        'w_in': nrm(ks[8], (DEPTH, D_MODEL, IN_COLS), D_MODEL ** -0.5),
        'b_gates': jnp.concatenate([b_i, b_f], axis=2),
        'mlstm_norm_g': 1.0 + nrm(ks[12], (DEPTH, MLSTM_WIDTH), 0.02),
        'gmlp_ws': nrm(ks[13], (DEPTH, GMLP_HEADS, GMLP_CHUNK, GMLP_CHUNK), GMLP_CHUNK ** -0.5),
        'gmlp_bs': 1.0 + nrm(ks[14], (DEPTH, GMLP_HEADS, GMLP_CHUNK), 0.02),
        'gmlp_norm_g': 1.0 + nrm(ks[15], (DEPTH, GMLP_WIDTH), 0.02),
        'conv_w': nrm(ks[16], (DEPTH, CONV_K, CONV_WIDTH), CONV_K ** -0.5),
        'w_out': nrm(ks[17], (DEPTH, MIX_WIDTH, D_MODEL), MIX_WIDTH ** -0.5),
        'w_router': nrm(ks[18], (D_MODEL, N_EXPERTS), D_MODEL ** -0.5),
        'b_router': nrm(ks[19], (N_EXPERTS,), 0.01),
        'w_gate_e': nrm(ks[20], (DEPTH, N_EXPERTS, D_MODEL, D_FF_EXPERT), D_MODEL ** -0.5),
        'w_up_e': nrm(ks[21], (DEPTH, N_EXPERTS, D_MODEL, D_FF_EXPERT), D_MODEL ** -0.5),
        'w_down_e': nrm(ks[22], (DEPTH, N_EXPERTS, D_FF_EXPERT, D_MODEL), D_FF_EXPERT ** -0.5),
        'final_g': 1.0 + nrm(ks[23], (D_MODEL,), 0.02),
    }


def reference(x, c, ctx, c_ctx, w_mod, b_mod, norm1_g, norm2_g, w_in, b_gates, mlstm_norm_g, gmlp_ws, gmlp_bs, gmlp_norm_g, conv_w, w_out, w_router, b_router, w_gate_e, w_up_e, w_down_e, final_g):
    rows = x.shape[1] // GRID_W
    for layer in range(DEPTH):
        mod_x = jnp.split((jax.nn.silu(c) @ w_mod[layer] + b_mod[layer])[:, None, :], N_MOD, axis=-1)
        mod_c = jnp.split(jax.nn.silu(c_ctx) @ w_mod[layer] + b_mod[layer], N_MOD, axis=-1)
        hx = modulate(rms_norm(x, norm1_g[layer]), mod_x[0], mod_x[1])
        hc = modulate(rms_norm(ctx, norm1_g[layer]), mod_c[0], mod_c[1])
        px = split_cols(hx @ w_in[layer])
        pc = split_cols(hc @ w_in[layer])
        mc, ctx_states = mlstm_mixer(pc[0], pc[1], pc[2], pc[3], pc[4], b_gates[layer], mlstm_norm_g[layer], None)
        mx, _ = mlstm_mixer(px[0], px[1], px[2], px[3], px[4], b_gates[layer], mlstm_norm_g[layer], ctx_states)
        yx = jnp.concatenate([
            mx,
            gmlp_mixer(px[5], px[6], gmlp_ws[layer], gmlp_bs[layer], gmlp_norm_g[layer]),
            conv_mixer(px[7], px[8], px[9], conv_w[layer], rows),
        ], axis=-1) @ w_out[layer]
        x_mix = x + mod_x[2] * yx
        x_new = x_mix + mod_x[5] * moe_ffn(modulate(rms_norm(x_mix, norm2_g[layer]), mod_x[3], mod_x[4]),
                                           w_router, b_router, w_gate_e[layer], w_up_e[layer], w_down_e[layer])
        if layer + 1 < DEPTH:
            yc = jnp.concatenate([
                mc,
                gmlp_mixer(pc[5], pc[6], gmlp_ws[layer], gmlp_bs[layer], gmlp_norm_g[layer]),
                conv_mixer(pc[7], pc[8], pc[9], conv_w[layer], None),
            ], axis=-1) @ w_out[layer]
            ctx_mix = ctx + mod_c[2] * yc
            ctx = ctx_mix + mod_c[5] * moe_ffn(modulate(rms_norm(ctx_mix, norm2_g[layer]), mod_c[3], mod_c[4]),
                                               w_router, b_router, w_gate_e[layer], w_up_e[layer], w_down_e[layer])
        x = x_new
    return rms_norm(x, final_g)
```

```python
from contextlib import ExitStack
import numpy as np
import concourse.bass as bass
import concourse.mybir as mybir
from concourse.bass_utils import run_bass_kernel_spmd

F32 = mybir.dt.float32
BF16 = mybir.dt.bfloat16
AF = mybir.ActivationFunctionType
ALU = mybir.AluOpType
EPS = 1e-6


class Prog:
    def __init__(self, nc):
        self.nc = nc
        self.ops = []
        self.last_writer = {}
        self.readers = {}
        self.multi_flag = {}
        self.multi_base = {}
        self.fence_deps = {}

    def fence(self):
        last = {}
        for i, o in enumerate(self.ops):
            if o['fn'] is None:
                continue
            k = ('d_' + str(o['key'])) if o['dma'] else ('e_' + o['eng'])
            last[k] = i
        tails = list(last.values())
        for d in tails:
            self.ops[d]['signal'] = True
        for eng in ('pe', 'act', 'dve', 'pool', 'sp'):
            self.ops.append(dict(eng=eng, fn=None, deps=set(tails), dma=False, key=None,
                                 signal=False, inc=1, final=False))
        self.fence_deps = {}

    def op(self, eng, fn, reads=(), writes=(), dma_key=None, inc=16, final=False, multi=False):
        i = len(self.ops)
        deps = set()
        for r in reads:
            for w in self.last_writer.get(r, ()):
                deps.add((w, 'raw'))
            rl = self.readers.setdefault(r, [])
            if dma_key is None:
                rl[:] = [q for q in rl if self.ops[q]['dma'] or self.ops[q]['eng'] != eng]
            rl.append(i)
        for r in writes:
            lw = self.last_writer.get(r, [])
            rd = self.readers.get(r, [])
            if multi and self.multi_flag.get(r, False) and not rd:
                lw.append(i)
                for d in self.multi_base.get(r, ()):
                    deps.add(d)
                continue
            base = set()
            for w in lw:
                base.add((w, 'waw'))
            for q in rd:
                if q != i:
                    base.add((q, 'war'))
            deps |= base
            self.last_writer[r] = [i]
            self.readers[r] = []
            self.multi_flag[r] = multi
            self.multi_base[r] = base
        for f in self.fence_deps.get(eng, ()):
            deps.add((f, 'fence'))
        is_dma = dma_key is not None
        keep = set()
        for d, kind in deps:
            o = self.ops[d]
            if o['fn'] is None:
                continue
            if o['eng'] == eng and not is_dma and not o['dma']:
                if eng == 'pe':
                    continue
                if kind == 'war':
                    continue
            keep.add(d)
        for d in keep:
            self.ops[d]['signal'] = True
        self.ops.append(dict(eng=eng, fn=fn, deps=keep, dma=is_dma, key=dma_key,
                             signal=is_dma, inc=(inc if is_dma else 1), final=final))
        return i

    def emit(self, stack):
        nc = self.nc
        engs = ['pe', 'act', 'dve', 'pool', 'sp']
        sems = {}
        counts = {}
        for o in self.ops:
            if not o['signal']:
                continue
            k = ('d_' + str(o['key'])) if o['dma'] else ('e_' + o['eng'])
            counts[k] = counts.get(k, 0) + o['inc']
            o['sem'] = k
            o['val'] = counts[k]
            if k not in sems:
                sems[k] = stack.enter_context(nc.semaphore(k))
        self.nsems = len(sems)
        per_eng = {e: [] for e in engs}
        for o in self.ops:
            per_eng[o['eng']].append(o)
        ops = self.ops
        final_waits = {}
        for o in ops:
            if o['dma'] and o['final']:
                final_waits[o['sem']] = max(final_waits.get(o['sem'], 0), o['val'])
        block = stack.enter_context(nc.Block())

        def run(engname, e):
            waited = {}
            for o in per_eng[engname]:
                need = {}
                for d in o['deps']:
                    od = ops[d]
                    need[od['sem']] = max(need.get(od['sem'], 0), od['val'])
                for s, v in need.items():
                    if waited.get(s, 0) < v:
                        e.wait_ge(sems[s], v)
                        waited[s] = v
                if o['fn'] is None:
                    continue
                inst = o['fn'](e)
                if o['signal']:
                    inst.then_inc(sems[o['sem']], o['inc'])
            if engname == 'sp':
                for s, v in final_waits.items():
                    e.wait_ge(sems[s], v)

        @block.tensor
        def _(e):
            run('pe', e)

        @block.scalar
        def _(e):
            run('act', e)

        @block.vector
        def _(e):
            run('dve', e)

        @block.gpsimd
        def _(e):
            run('pool', e)

        @block.sync
        def _(e):
            run('sp', e)


class Cfg:
    def __init__(self, D=4096, DEPTH=2, BATCH=2, SEG=4, NL=8, DFF=None, dbg=None, stop=None):
        self.D, self.DEPTH, self.BATCH, self.SEG, self.NL = D, DEPTH, BATCH, SEG, NL
        self.NCORES = BATCH * SEG
        self.CTXC = 2
        self.NCH = NL + 2
        self.T = self.NCH * 128
        self.TL = NL * 128
        self.KD = D // 128
        self.H = 4
        self.MW = D // 2
        self.DV = self.MW // 4
        self.DK = self.DV // 2
        self.QKW = 4 * self.DK
        self.DKc = min(128, self.DK)
        self.KC = self.DK // self.DKc
        self.GW = D // 4
        self.GHD = self.GW // 4
        self.CW = D // 4
        self.NE, self.NG, self.EPG = 16, 4, 4
        self.DFF = DFF if DFF is not None else D // 4
        self.FB = self.DFF // 128
        o = 0
        self.off = {}
        for n, w in (('q', self.QKW), ('k', self.QKW), ('v', self.MW), ('o', self.MW), ('g', 16),
                     ('gu', self.GW), ('gv', self.GW), ('cb', self.CW), ('cc', self.CW), ('cx', self.CW)):
            self.off[n] = o
            o += w
        self.INC = o
        self.dbg = dbg or []
        self.stop = stop
        self.c_id, self.c_U, self.c_L, self.c_one = 0, 128, 256, 384
        self.c_mL = 512
        self.c_mR = 512 + self.T
        self.NCONST = 512 + 2 * self.T
        self.NST = 2 * 4 * self.KC * (self.DV + 1)


def consts_array(cfg):
    c = np.zeros((128, cfg.NCONST), np.float32)
    c[:, 0:128] = np.eye(128, dtype=np.float32)
    s = np.arange(128)
    c[:, 128:256] = (s[:, None] <= s[None, :]).astype(np.float32)
    c[:, 256:384] = (s[:, None] >= s[None, :]).astype(np.float32)
    c[:, 384:512] = 1.0
    T = cfg.T
    mL = np.ones(T, np.float32)
    mR = np.ones(T, np.float32)
    mL[0] = 0
    mR[255] = 0
    for p in range(256, T):
        if (p - 256) % 64 == 0:
            mL[p] = 0
        if (p - 256) % 64 == 63:
            mR[p] = 0
    c[:, cfg.c_mL:cfg.c_mL + T] = mL[None, :]
    c[:, cfg.c_mR:cfg.c_mR + T] = mR[None, :]
    return c


def percore_array(cfg, core):
    NR = cfg.NCORES
    b, r = divmod(core, cfg.SEG)
    Mx = np.zeros((128, (NR + 1) * 8), np.float32)
    valid = np.zeros((128, (NR + 1) * 8), np.float32)
    for g in range(NR + 1):
        for d in range(2):
            for h in range(4):
                col = g * 8 + d * 4 + h
                if g < NR:
                    bg, rg = divmod(g, cfg.SEG)
                    ok = (bg == b) and ((rg < r) if d == 0 else (rg > r))
                else:
                    ok = True
                    bg, rg = b, (-1 if d == 0 else cfg.SEG)
                valid[:, col] = 1.0 if ok else 0.0
                if ok:
                    for g2 in range(NR):
                        b2, r2 = divmod(g2, cfg.SEG)
                        if b2 != b:
                            continue
                        between = (rg < r2 < r) if d == 0 else (r < r2 < rg)
                        if between:
                            Mx[g2, col] = 1.0
    return np.concatenate([Mx, valid], axis=1)


class Builder:
    def __init__(self, cfg):
        self.cfg = cfg
        self.nc = bass.Bass("TRN2", target_bir_lowering=False)
        self.P = Prog(self.nc)
        self.uid = 0

    def dram(self, name, shape, dt, kind=None):
        if kind is None and name in self.cfg.dbg:
            kind = "ExternalOutput"
        if kind is None:
            return self.nc.dram_tensor(name, list(shape), dt).ap()
        return self.nc.dram_tensor(name, list(shape), dt, kind=kind).ap()

    def sb(self, ph, name, shape, dt):
        self.uid += 1
        return ph.enter_context(self.nc.sbuf_tensor(f'{name}_s{self.uid}', list(shape), dt))

    def ps(self, ph, name, shape, dt=F32):
        self.uid += 1
        esz = 4 if dt == F32 else 2
        n = 1
        for d in shape[1:]:
            n *= d
        per_bank = 2048 // esz
        n = ((n + per_bank - 1) // per_bank) * per_bank
        return ph.enter_context(self.nc.psum_tensor(f'{name}_p{self.uid}', [128, n], dt))

    def dma(self, q, out, in_, reads, writes, key, final=False, multi=False):
        return self.P.op(q, lambda e: e.dma_start(out=out, in_=in_), reads=reads, writes=writes,
                         dma_key=key, final=final, multi=multi)

    def mm(self, out, lhsT, rhs, start, stop, reads, writes):
        return self.P.op('pe', lambda e: e.matmul(out, lhsT, rhs, start=start, stop=stop),
                         reads=reads, writes=writes)

    def tr(self, out, in_, ident, reads, writes):
        return self.P.op('pe', lambda e: e.transpose(out, in_, ident), reads=reads, writes=writes)

    def act(self, out, in_, func, reads, writes, bias=None, scale=None, accum_out=None):
        kw = {}
        if bias is not None:
            kw['bias'] = bias
        if scale is not None:
            kw['scale'] = scale
        if accum_out is not None:
            kw['accum_out'] = accum_out
        return self.P.op('act', lambda e: e.activation(out=out, in_=in_, func=func, **kw),
                         reads=reads, writes=writes)

    def tt(self, out, a, b, op, reads, writes, eng='dve'):
        return self.P.op(eng, lambda e: e.tensor_tensor(out, a, b, op), reads=reads, writes=writes)

    def ts(self, out, a, s1, s2, op0, op1, reads, writes, eng='dve'):
        if s2 is None:
            return self.P.op(eng, lambda e: e.tensor_scalar(out, a, s1, None, op0), reads=reads, writes=writes)
        return self.P.op(eng, lambda e: e.tensor_scalar(out, a, s1, s2, op0, op1), reads=reads, writes=writes)

    def stt(self, out, a, s, b, op0, op1, reads, writes, eng='dve'):
        return self.P.op(eng, lambda e: e.scalar_tensor_tensor(out, a, s, b, op0, op1), reads=reads, writes=writes)

    def cp(self, out, a, reads, writes, eng='dve'):
        return self.P.op(eng, lambda e: e.tensor_copy(out, a), reads=reads, writes=writes)

    def rstd(self, out, tmp, sumsq, inv_n, key):
        self.ts(tmp, sumsq, inv_n, EPS, ALU.mult, ALU.add, [key], [key])
        self.act(tmp, tmp, AF.Sqrt, [key], [key])
        self.P.op('dve', lambda e: e.reciprocal(out, tmp), reads=[key], writes=[key])

    def memset(self, out, v, writes, eng='dve'):
        return self.P.op(eng, lambda e: e.memset(out, v), writes=writes)

    def load_w(self, wt, slot, key, W2d, kch, col0, ncols):
        src = W2d[:, col0:col0 + ncols].rearrange("(k p) n -> p k n", p=128)
        step = 8
        for k0 in range(0, kch, step):
            k1 = min(kch, k0 + step)
            self.dma('pool', wt[slot][:, k0:k1, 0:ncols], src[:, k0:k1, :], reads=[], writes=[f'{key}{slot}'],
                     key=f'{key}{slot}', multi=True)

    def build(self):
        cfg, nc = self.cfg, self.nc
        D, T, KD, DEPTH = cfg.D, cfg.T, cfg.KD, cfg.DEPTH
        NR = cfg.NCORES
        I = {}
        def inp(name, shape):
            I[name] = nc.dram_tensor(name, list(shape), F32, kind="ExternalInput").ap()
        inp('xin', [T, D]); inp('cvec', [2, D]); inp('consts', [128, cfg.NCONST])
        inp('percore', [128, 2 * (NR + 1) * 8])
        inp('w_mod', [DEPTH * D, 6 * D]); inp('b_mod', [DEPTH, 6 * D])
        inp('norm1_g', [DEPTH, D]); inp('norm2_g', [DEPTH, D])
        inp('w_in', [DEPTH * D, cfg.INC]); inp('b_gates', [DEPTH, 16])
        inp('mlstm_norm_g', [DEPTH, cfg.MW]); inp('gmlp_ws', [DEPTH * 4 * 128, 128]); inp('gmlp_bs', [DEPTH * 4, 128])
        inp('gmlp_norm_g', [DEPTH, cfg.GW]); inp('conv_w', [DEPTH * 3, cfg.CW]); inp('w_out', [DEPTH * D, D])
        inp('w_router', [D, 16]); inp('b_router', [1, 16])
        inp('w_gate_e', [DEPTH * 16 * D, cfg.DFF]); inp('w_up_e', [DEPTH * 16 * D, cfg.DFF])
        inp('w_down_e', [DEPTH * 16 * cfg.DFF, D]); inp('final_g', [1, D])
        self.I = I
        self.y = nc.dram_tensor('y', [cfg.TL, D], F32, kind="ExternalOutput").ap()
        S = {}
        S['xa'] = self.dram('xa', [T, D], F32)
        S['xmix'] = self.dram('xmix', [T, D], F32)
        S['mod'] = self.dram('mod', [2, 6 * D], F32)
        S['qT'] = self.dram('qT', [cfg.QKW, T], BF16)
        S['kT'] = self.dram('kT', [cfg.QKW, T], BF16)
        S['kt'] = self.dram('kt', [T, cfg.QKW], BF16)
        S['v'] = self.dram('v', [T, cfg.MW], BF16)
        S['o'] = self.dram('o', [T, cfg.MW], F32)
        S['g'] = self.dram('g', [T, 16], F32)
        S['gu'] = self.dram('gu', [T, cfg.GW], F32)
        S['gv'] = self.dram('gv', [T, cfg.GW], F32)
        for n in ('cb', 'cc', 'cx'):
            S[n] = self.dram(n, [cfg.CW, T], F32)
        S['hf'] = self.dram('hf', [T, cfg.MW], F32)
        S['ymT'] = self.dram('ymT', [D, T], BF16)
        S['h2T'] = self.dram('h2T', [cfg.NCH * 128, KD * 128], BF16)
        S['send'] = self.dram('send', [128, cfg.NST + 8], F32)
        S['gath'] = self.dram('gath', [NR * 128, cfg.NST + 8], F32)
        S['cst'] = self.dram('cst', [128, cfg.NST], F32)
        S['aT'] = self.dram('aTd', [16 * cfg.FB * 128, T], BF16)
        self.S = S
        with ExitStack() as st:
            self.st = st
            cst = self.sb(st, 'consts', [128, cfg.NCONST], F32)
            self.dma('sp', cst[:, :], I['consts'][:, :], [], ['consts'], 'consts')
            self.cst = cst
            idb = self.sb(st, 'idb', [128, 128], BF16)
            self.cp(idb[:, :], cst[:, 0:128], ['consts'], ['idb'])
            self.idb = idb
            cur = I['xin']
            for l in range(DEPTH):
                last = (l == DEPTH - 1)
                self.phase_mod(l)
                if cfg.stop == f'mod{l}':
                    break
                self.phase_win(l, cur)
                if cfg.stop == f'win{l}':
                    break
                self.phase_mlstm(l)
                if cfg.stop == f'mlstm{l}':
                    break
                self.phase_gmlp(l)
                self.phase_conv(l)
                if cfg.stop == f'mix{l}':
                    break
                self.phase_wout(l, cur)
                if cfg.stop == f'wout{l}':
                    break
                self.phase_moe(l, last)
                if cfg.stop in (f'rt{l}', f'n2{l}'):
                    break
                cur = S['xa']
            if cfg.stop is not None:
                with ExitStack() as ph:
                    z = self.sb(ph, 'zz', [128, D], F32)
                    self.memset(z[:, :], 0.0, ['zz'])
                    for tb in range(cfg.NL):
                        self.dma('sp', self.y[tb * 128:(tb + 1) * 128, :], z[:, :], ['zz'], ['y'], 'yout', final=True)
            self.P.emit(st)
        return nc

    def phase_mod(self, l):
        cfg, I, S = self.cfg, self.I, self.S
        D, KD = cfg.D, cfg.KD
        W = I['w_mod'][l * D:(l + 1) * D, :]
        with ExitStack() as ph:
            craw = self.sb(ph, 'craw', [2 * KD, 128], F32)
            for v in range(2):
                self.dma('sp', craw[v * KD:(v + 1) * KD, :], I['cvec'][v:v + 1, :].rearrange("o (k p) -> (o k) p", p=128),
                         [], ['craw'], 'craw')
            pT = self.ps(ph, 'mod_pT', [128, 2 * KD], F32)
            self.tr(pT[:, 0:2 * KD], craw[:, :], self.cst[0:2 * KD, 0:2 * KD], ['craw', 'consts'], ['mod_pT'])
            sc = self.sb(ph, 'sc', [128, KD, 2], BF16)
            for v in range(2):
                self.act(sc[:, :, v], pT[:, v * KD:(v + 1) * KD], AF.Silu, ['mod_pT'], ['sc'])
            bm = [self.sb(ph, f'bm{s}', [2, 512], F32) for s in range(2)]
            mo = [self.sb(ph, f'mo{s}', [2, 512], F32) for s in range(2)]
            wt = [self.sb(ph, f'modw{s}', [128, KD, 512], BF16) for s in range(2)]
            pacc = [self.ps(ph, f'mod_ps{s}', [2, 512], F32) for s in range(2)]
            nblk = 6 * D // 512
            for j in range(nblk):
                s = j % 2
                self.load_w(wt, s, 'modw', W, KD, j * 512, 512)
                self.dma('sp', bm[s][:, :], I['b_mod'][l:l + 1, j * 512:(j + 1) * 512].partition_broadcast(2), [], [f'bm{s}'], f'bm{s}')
                for k in range(KD):
                    self.mm(pacc[s][0:2, 0:512], sc[:, k, :], wt[s][:, k, :], k == 0, k == KD - 1,
                            ['sc', f'modw{s}'], [f'mod_ps{s}'])
                self.tt(mo[s][:, :], pacc[s][0:2, 0:512], bm[s][:, :], ALU.add, [f'mod_ps{s}', f'bm{s}'], [f'mo{s}'])
                self.dma('sp', S['mod'][:, j * 512:(j + 1) * 512], mo[s][:, :], [f'mo{s}'], ['mod_d'], f'mod_d{s}', multi=True)
        self.P.fence()

    def load_mod_bc(self, ph, name, l, idx, who, plus_one=False, gain=None):
        cfg, I, S = self.cfg, self.I, self.S
        D = cfg.D
        t = self.sb(ph, name, [128, D], F32)
        self.dma('sp', t[:, :], S['mod'][who:who + 1, idx * D:(idx + 1) * D].partition_broadcast(128),
                 ['mod_d'], [name], name)
        if plus_one:
            with ExitStack() as tmp:
                g = self.sb(tmp, name + '_g', [128, D], F32)
                self.dma('sp', g[:, :], gain.partition_broadcast(128), [], [name + '_g'], name + '_g')
                self.stt(t[:, :], t[:, :], 1.0, g[:, :], ALU.add, ALU.mult, [name, name + '_g'], [name])
            self.P.fence()
        return t

    def norm_mod_T(self, ph, tag, src, tb, A, Bt, An, Bn, hT, hTkey, col0, xt, sq, keep32=None):
        cfg = self.cfg
        D, KD = cfg.D, cfg.KD
        s = tb % 2
        x = xt[s]
        self.dma('sp', x[:, :], src[tb * 128:(tb + 1) * 128, :], [], [f'{tag}x{s}'], f'{tag}x{s}')
        ss = sq['ss']
        self.memset(ss[:, 0:1], 0.0, [f'{tag}ss'])
        self.act(sq['junk'][:, :], x[:, :], AF.Square, [f'{tag}x{s}'], [f'{tag}junk', f'{tag}ss'], accum_out=ss[:, 0:1])
        self.rstd(ss[:, 2:3], ss[:, 1:2], ss[:, 0:1], 1.0 / D, f'{tag}ss')
        self.stt(x[:, :], x[:, :], ss[:, 2:3], A[:, :], ALU.mult, ALU.mult, [f'{tag}x{s}', f'{tag}ss', An], [f'{tag}x{s}'])
        self.tt(x[:, :], x[:, :], Bt[:, :], ALU.add, [f'{tag}x{s}', Bn], [f'{tag}x{s}'])
        pt = sq['pt']
        for k0 in range(0, KD, 4):
            pi = (k0 // 4) % 2
            for k in range(k0, min(KD, k0 + 4)):
                self.tr(pt[pi][:, (k - k0) * 128:(k - k0 + 1) * 128], x[:, k * 128:(k + 1) * 128], self.cst[:, 0:128],
                        [f'{tag}x{s}', 'consts'], [f'{tag}pt{pi}'])
            n = min(KD, k0 + 4) - k0
            self.P.op('act', lambda e, pi=pi, k0=k0, n=n: e.activation(
                out=hT[:, k0:k0 + n, col0:col0 + 128], in_=pt[pi][:, 0:n * 128].rearrange("p (k t) -> p k t", t=128),
                func=AF.Copy), reads=[f'{tag}pt{pi}'], writes=[hTkey, f'{tag}ptlock{pi}'])
            if keep32 is not None:
                self.P.op('dve', lambda e, pi=pi, k0=k0, n=n: e.tensor_copy(
                    keep32[:, k0:k0 + n, :], pt[pi][:, 0:n * 128].rearrange("p (k t) -> p k t", t=128)),
                    reads=[f'{tag}pt{pi}'], writes=[f'{tag}k32', f'{tag}ptlock{pi}'])

    def phase_win(self, l, cur):
        cfg, I, S = self.cfg, self.I, self.S
        D, T, KD, NCH = cfg.D, cfg.T, cfg.KD, cfg.NCH
        W = I['w_in'][l * D:(l + 1) * D, :]
        with ExitStack() as ph:
            hT = self.sb(ph, 'hT', [128, KD, T], BF16)
            with ExitStack() as p2:
                A = [self.load_mod_bc(p2, f'n1A{w}', l, 1, w, True, I['norm1_g'][l:l + 1, :]) for w in range(2)]
                Bt = [self.load_mod_bc(p2, f'n1B{w}', l, 0, w) for w in range(2)]
                xt = [self.sb(p2, f'n1x{s}', [128, D], F32) for s in range(2)]
                sq = dict(ss=self.sb(p2, 'n1ss', [128, 4], F32), junk=self.sb(p2, 'n1junk', [128, D], BF16),
                          pt=[self.ps(p2, f'n1pt{i}', [128, 512], F32) for i in range(2)])
                for tb in range(NCH):
                    w = 1 if tb < 2 else 0
                    self.norm_mod_T(p2, 'n1', cur, tb, A[w], Bt[w], f'n1A{w}', f'n1B{w}', hT, 'hT', tb * 128, xt, sq)
            self.P.fence()
            wt = [self.sb(ph, f'winw{s}', [128, KD, 512], BF16) for s in range(2)]
            pacc = [self.ps(ph, f'win_ps{s}', [128, 512], F32) for s in range(4)]
            ev = [self.sb(ph, f'win_ev{s}', [128, 512], F32) for s in range(4)]
            evb = [self.sb(ph, f'win_evb{s}', [128, 512], BF16) for s in range(4)]
            self.wctr = 0
            self.pctr = 0
            full = (l == 0)
            def tblocks(name):
                if full or name in ('k', 'v', 'g'):
                    return list(range(NCH))
                return list(range(2, NCH))
            def gemm_T(name, dst, dt, func=None, scale=None):
                c0, n = cfg.off[name], {'k': cfg.QKW, 'v': cfg.MW, 'o': cfg.MW, 'g': 16, 'gu': cfg.GW, 'gv': cfg.GW}[name]
                for cb in range(0, n, 512):
                    w = min(512, n - cb)
                    s = self.wctr % 2
                    self.wctr += 1
                    self.load_w(wt, s, 'winw', W, KD, c0 + cb, w)
                    for tb in tblocks(name):
                        p = self.pctr % 4
                        self.pctr += 1
                        for k in range(KD):
                            self.mm(pacc[p][:, 0:w], hT[:, k, tb * 128:(tb + 1) * 128], wt[s][:, k, 0:w], k == 0, k == KD - 1,
                                    ['hT', f'winw{s}'], [f'win_ps{p}'])
                        o = (evb if dt == BF16 else ev)[p]
                        okey = f'win_ev{"b" if dt == BF16 else ""}{p}'
                        if func is not None:
                            self.act(o[:, 0:w], pacc[p][:, 0:w], func, [f'win_ps{p}'], [okey])
                        else:
                            self.cp(o[:, 0:w], pacc[p][:, 0:w], [f'win_ps{p}'], [okey])
                        self.dma('sp', dst[tb * 128:(tb + 1) * 128, cb:cb + w], o[:, 0:w], [okey], [name + '_d'], okey + 'st', multi=True)
            def gemm_F(name, dst, dt, scale=None):
                c0, n = cfg.off[name], {'q': cfg.QKW, 'k': cfg.QKW, 'cb': cfg.CW, 'cc': cfg.CW, 'cx': cfg.CW}[name]
                t0s = 0 if (full or name == 'k') else 256
                for cb in range(0, n, 512):
                    w = min(512, n - cb)
                    s = self.wctr % 2
                    self.wctr += 1
                    self.load_w(wt, s, 'winw', W, KD, c0 + cb, w)
                    for cc in range(0, w, 128):
                        cw = min(128, w - cc)
                        for t0 in range(t0s, T, 512):
                            tn = min(512, T - t0)
                            p = self.pctr % 4
                            self.pctr += 1
                            for k in range(KD):
                                self.mm(pacc[p][0:cw, 0:tn], wt[s][:, k, cc:cc + cw], hT[:, k, t0:t0 + tn], k == 0, k == KD - 1,
                                        ['hT', f'winw{s}'], [f'win_ps{p}'])
                            o = (evb if dt == BF16 else ev)[p]
                            okey = f'win_ev{"b" if dt == BF16 else ""}{p}'
                            if scale is not None:
                                self.act(o[0:cw, 0:tn], pacc[p][0:cw, 0:tn], AF.Copy, [f'win_ps{p}'], [okey], scale=scale)
                            else:
                                self.cp(o[0:cw, 0:tn], pacc[p][0:cw, 0:tn], [f'win_ps{p}'], [okey])
                            self.dma('sp', dst[cb + cc:cb + cc + cw, t0:t0 + tn], o[0:cw, 0:tn], [okey], [name + '_d'], okey + 'st', multi=True)
            gemm_F('q', S['qT'], BF16, scale=float(cfg.DK) ** -0.5)
            gemm_F('k', S['kT'], BF16)
            gemm_T('k', S['kt'], BF16)
            gemm_T('v', S['v'], BF16)
            gemm_T('o', S['o'], F32, func=AF.Sigmoid)
            gemm_T('g', S['g'], F32)
            gemm_T('gu', S['gu'], F32)
            gemm_T('gv', S['gv'], F32)
            gemm_F('cb', S['cb'], F32)
            gemm_F('cc', S['cc'], F32)
            gemm_F('cx', S['cx'], F32)
        self.P.fence()

    def phase_mlstm(self, l):
        cfg, I, S = self.cfg, self.I, self.S
        D, T, KD, NCH, H, DV, DK, DKc, KC, MW = cfg.D, cfg.T, cfg.KD, cfg.NCH, cfg.H, cfg.DV, cfg.DK, cfg.DKc, cfg.KC, cfg.MW
        NR = cfg.NCORES
        cst = self.cst
        full = (l == 0)
        with ExitStack() as ph:
            C32 = self.sb(ph, 'C32', [128, 2 * H * KC, DV], F32)
            Cb = self.sb(ph, 'Cb', [128, 2 * H * KC, DV], BF16)
            N32 = self.sb(ph, 'N32', [128, 2 * H * KC], F32)
            Nb = self.sb(ph, 'Nb', [128, 2 * H * KC], BF16)
            Bt = self.sb(ph, 'Btot', [128, 8], F32)
            bgt = self.sb(ph, 'bgt', [128, 16], F32)
            self.dma('sp', bgt[:, :], I['b_gates'][l:l + 1, :].partition_broadcast(128), [], ['bgt'], 'bgt')
            g = self.sb(ph, 'gch', [128, 16], F32)
            z = self.sb(ph, 'zch', [128, 16], F32)
            sp_ = self.sb(ph, 'spch', [128, 8], F32)
            wv = self.sb(ph, 'wch', [128, 8], F32)
            ej = self.sb(ph, 'ejch', [128, 8], F32)
            et = self.sb(ph, 'etch', [128, 8], F32)
            etk = self.sb(ph, 'etk', [128, 8 * KC], F32)
            tmp8 = self.sb(ph, 'tmp8', [128, 8], F32)
            gsb = self.sb(ph, 'gsb', [128, 16], F32)
            kt = self.sb(ph, 'ktch', [128, cfg.QKW], BF16)
            vt = self.sb(ph, 'vch', [128, MW], BF16)
            kT = self.sb(ph, 'kTch', [128, H * KC, 128], BF16)
            qT = self.sb(ph, 'qTch', [128, H * KC, 128], BF16)
            VS = [self.sb(ph, f'VS{i}', [128, DV + 1], BF16) for i in range(2)]
            Sm = [self.sb(ph, f'Sm{i}', [128, 128], BF16) for i in range(2)]
            hfl = self.sb(ph, 'hfl', [128, MW], F32)
            hs = self.sb(ph, 'hsum', [128, MW], F32)
            osg = self.sb(ph, 'osg', [128, MW], F32)
            yb = self.sb(ph, 'ymb', [128, MW], BF16)
            ymT = self.sb(ph, 'ymTt', [128, MW // 128, 128], BF16)
            ngb = self.sb(ph, 'mngb', [128, MW], F32)
            self.dma('sp', ngb[:, :], I['mlstm_norm_g'][l:l + 1, :].partition_broadcast(128), [], ['mngb'], 'mngb')
            sm4 = self.sb(ph, 'sm4', [128, 16], F32)
            junk = self.sb(ph, 'mjunk', [128, DV], BF16)
            ctmp = self.sb(ph, 'ctmp', [128, DV], F32)
            ps_s = self.ps(ph, 'ps_s', [128, 128], F32)
            ps_n = self.ps(ph, 'ps_n', [128, 512], F32)
            ps_d = self.ps(ph, 'ps_d', [128, 16], F32)
            ps_c = [self.ps(ph, f'ps_c{i}', [128, 512], F32) for i in range(2)]
            ps_cn = self.ps(ph, 'ps_cn', [128, 16], F32)
            ps_g = self.ps(ph, 'ps_g', [128, 32], F32)
            ps_t = self.ps(ph, 'ps_t', [128, 1024], BF16)

            def zero_state():
                self.memset(C32[:, :, :], 0.0, ['C32'])
                self.memset(Cb[:, :, :], 0.0, ['Cb'])
                self.memset(N32[:, :], 0.0, ['N32'])
                self.memset(Nb[:, :], 0.0, ['Nb'])
                self.memset(Bt[:, :], 0.0, ['Btot'])

            def prologue(c, d):
                r0 = c * 128
                self.dma('sp', g[:, :], S['g'][r0:r0 + 128, :], ['g_d'], ['gch'], 'gch')
                self.tt(z[:, :], g[:, :], bgt[:, :], ALU.add, ['gch', 'bgt'], ['zch'])
                zi = z[:, d * 8:d * 8 + 4]
                zf = z[:, d * 8 + 4:d * 8 + 8]
                dd = slice(d * 4, d * 4 + 4)
                self.act(tmp8[:, dd], zf, AF.Exp, ['zch'], ['tmp8'], scale=-1.0)
                self.act(sp_[:, dd], tmp8[:, dd], AF.Ln, ['tmp8'], ['spch'], bias=1.0)
                mask = cst[:, cfg.c_U:cfg.c_U + 128] if d == 0 else cst[:, cfg.c_L:cfg.c_L + 128]
                self.mm(ps_g[:, 0:4], mask, sp_[:, dd], True, True, ['consts', 'spch'], ['ps_g'])
                self.mm(ps_g[:, 8:12], cst[:, cfg.c_one:cfg.c_one + 128], sp_[:, dd], True, True, ['consts', 'spch'], ['ps_g'])
                self.cp(gsb[:, 0:16], ps_g[:, 0:16], ['ps_g'], ['gsb'])
                self.tt(tmp8[:, dd], zi, gsb[:, 0:4], ALU.add, ['zch', 'gsb'], ['tmp8'])
                self.act(wv[:, dd], tmp8[:, dd], AF.Exp, ['tmp8'], ['wch'])
                self.act(ej[:, dd], gsb[:, 0:4], AF.Exp, ['gsb'], ['ejch'], scale=-1.0)
                self.act(et[:, dd], gsb[:, 8:12], AF.Exp, ['gsb'], ['etch'], scale=-1.0)
                for kc in range(KC):
                    self.P.op('dve', lambda e, kc=kc: e.tensor_copy(
                        etk[:, d * 4 * KC:(d + 1) * 4 * KC].rearrange("p (h k) -> p h k", k=KC)[:, :, kc], et[:, dd]),
                        reads=['etch'], writes=['etk'])
                self.tt(Bt[:, dd], Bt[:, dd], gsb[:, 8:12], ALU.add, ['gsb', 'Btot'], ['Btot'])

            def chunk(c, d, outputs):
                r0 = c * 128
                prologue(c, d)
                self.dma('sp', kt[:, :], S['kt'][r0:r0 + 128, :], ['k_d'], ['ktch'], 'ktch')
                self.dma('sp', vt[:, :], S['v'][r0:r0 + 128, :], ['v_d'], ['vch'], 'vch')
                if outputs:
                    self.dma('sp', kT[0:DKc, :, :], S['kT'][:, r0:r0 + 128].rearrange("(j p) t -> p j t", p=DKc),
                             ['k_d'], ['kTch'], 'kTch')
                    self.dma('sp', qT[0:DKc, :, :], S['qT'][:, r0:r0 + 128].rearrange("(j p) t -> p j t", p=DKc),
                             ['q_d'], ['qTch'], 'qTch')
                    if d == 1:
                        self.dma('sp', hfl[:, :], S['hf'][r0:r0 + 128, :], ['hf_d'], ['hfl'], 'hfl')
                        self.dma('sp', osg[:, :], S['o'][r0:r0 + 128, :], ['o_d'], ['osg'], 'osg')
                mask = cst[:, cfg.c_U:cfg.c_U + 128] if d == 0 else cst[:, cfg.c_L:cfg.c_L + 128]
                for h in range(H):
                    dh = d * 4 + h
                    vi = h % 2
                    vs = VS[vi]
                    vk = f'VS{vi}'
                    self.ts(vs[:, 0:DV], vt[:, h * DV:(h + 1) * DV], wv[:, dh:dh + 1], None, ALU.mult, None, ['vch', 'wch'], [vk])
                    self.cp(vs[:, DV:DV + 1], wv[:, dh:dh + 1], ['wch'], [vk], eng='act' if False else 'dve')
                    if outputs:
                        sm = Sm[vi]
                        for kc in range(KC):
                            self.mm(ps_s[:, 0:128], kT[0:DKc, h * KC + kc, :], qT[0:DKc, h * KC + kc, :], kc == 0, kc == KC - 1,
                                    ['kTch', 'qTch'], ['ps_s'])
                        self.tt(sm[:, :], ps_s[:, 0:128], mask, ALU.mult, ['ps_s', 'consts'], [f'Sm{vi}'])
                        self.mm(ps_n[:, 0:DV], sm[:, :], vs[:, 0:DV], True, False, [f'Sm{vi}', vk], ['ps_n'])
                        for kc in range(KC):
                            self.mm(ps_n[:, 0:DV], qT[0:DKc, h * KC + kc, :], Cb[0:DKc, dh * KC + kc, :], False, kc == KC - 1,
                                    ['qTch', 'Cb'], ['ps_n'])
                        self.mm(ps_d[:, h:h + 1], sm[:, :], vs[:, DV:DV + 1], True, False, [f'Sm{vi}', vk], ['ps_d'])
                        for kc in range(KC):
                            self.mm(ps_d[:, h:h + 1], qT[0:DKc, h * KC + kc, :], Nb[0:DKc, dh * KC + kc:dh * KC + kc + 1], False,
                                    kc == KC - 1, ['qTch', 'Nb'], ['ps_d'])
                        self.tt(sm4[:, 0:1], ps_d[:, h:h + 1], ej[:, dh:dh + 1], ALU.mult, ['ps_d', 'ejch'], ['sm4'])
                        self.ts(sm4[:, 1:2], sm4[:, 0:1], -1.0, None, ALU.mult, None, ['sm4'], ['sm4'])
                        self.tt(sm4[:, 1:2], sm4[:, 1:2], sm4[:, 0:1], ALU.max, ['sm4'], ['sm4'])
                        self.ts(sm4[:, 1:2], sm4[:, 1:2], 1.0, None, ALU.max, None, ['sm4'], ['sm4'])
                        self.P.op('dve', lambda e: e.reciprocal(sm4[:, 2:3], sm4[:, 1:2]), reads=['sm4'], writes=['sm4'])
                        self.tt(sm4[:, 3:4], sm4[:, 2:3], ej[:, dh:dh + 1], ALU.mult, ['sm4', 'ejch'], ['sm4'])
                        if d == 0:
                            self.ts(hs[:, h * DV:(h + 1) * DV], ps_n[:, 0:DV], sm4[:, 3:4], None, ALU.mult, None, ['ps_n', 'sm4'], ['hsum'])
                        else:
                            self.stt(hs[:, h * DV:(h + 1) * DV], ps_n[:, 0:DV], sm4[:, 3:4], hfl[:, h * DV:(h + 1) * DV],
                                     ALU.mult, ALU.add, ['ps_n', 'sm4', 'hfl'], ['hsum'])
                    for kc in range(KC):
                        lk = kt[:, h * DK + kc * DKc:h * DK + (kc + 1) * DKc]
                        self.mm(ps_c[kc][0:DKc, 0:DV], lk, vs[:, 0:DV], True, True, ['ktch', vk], [f'ps_c{kc}'])
                        self.mm(ps_cn[0:DKc, h * KC + kc:h * KC + kc + 1], lk, vs[:, DV:DV + 1], True, True, ['ktch', vk], ['ps_cn'])
                    for kc in range(KC):
                        j = dh * KC + kc
                        self.tt(ctmp[0:DKc, :], ps_c[kc][0:DKc, 0:DV], C32[0:DKc, j, :], ALU.add, [f'ps_c{kc}', 'C32'], ['ctmp'])
                        self.ts(C32[0:DKc, j, :], ctmp[0:DKc, :], et[0:DKc, dh:dh + 1], None, ALU.mult, None, ['ctmp', 'etch'], ['C32'])
                        self.cp(Cb[0:DKc, j, :], C32[0:DKc, j, :], ['C32'], ['Cb'], eng='pool')
                jj = slice(d * 4 * KC, (d + 1) * 4 * KC)
                self.tt(N32[0:DKc, jj], N32[0:DKc, jj], ps_cn[0:DKc, 0:4 * KC], ALU.add, ['ps_cn', 'N32'], ['N32'])
                self.tt(N32[0:DKc, jj], N32[0:DKc, jj], etk[0:DKc, jj], ALU.mult, ['N32', 'etk'], ['N32'])
                self.cp(Nb[0:DKc, jj], N32[0:DKc, jj], ['N32'], ['Nb'])
                if outputs and d == 0:
                    self.dma('sp', S['hf'][r0:r0 + 128, :], hs[:, :], ['hsum'], ['hf_d'], 'hfst', multi=True)
                if outputs and d == 1:
                    self.memset(sm4[:, 4:8], 0.0, ['sm4'])
                    for h in range(H):
                        self.act(junk[:, :], hs[:, h * DV:(h + 1) * DV], AF.Square, ['hsum'], ['mjunk', 'sm4'],
                                 accum_out=sm4[:, 4 + h:5 + h])
                    self.rstd(sm4[:, 12:16], sm4[:, 8:12], sm4[:, 4:8], 1.0 / DV, 'sm4')
                    for h in range(H):
                        self.stt(hs[:, h * DV:(h + 1) * DV], hs[:, h * DV:(h + 1) * DV], sm4[:, 12 + h:13 + h],
                                 ngb[:, h * DV:(h + 1) * DV], ALU.mult, ALU.mult, ['hsum', 'sm4', 'mngb'], ['hsum'])
                    self.tt(yb[:, :], hs[:, :], osg[:, :], ALU.mult, ['hsum', 'osg'], ['ymb'])
                    nt = MW // 128
                    for j0 in range(0, nt, 8):
                        n = min(8, nt - j0)
                        for j in range(j0, j0 + n):
                            self.tr(ps_t[:, (j - j0) * 128:(j - j0 + 1) * 128], yb[:, j * 128:(j + 1) * 128], self.idb[:, :],
                                    ['ymb', 'idb'], ['ps_t'])
                        self.P.op('act', lambda e, j0=j0, n=n: e.activation(
                            out=ymT[:, j0:j0 + n, :], in_=ps_t[:, 0:n * 128].rearrange("p (k t) -> p k t", t=128), func=AF.Copy),
                            reads=['ps_t'], writes=['ymTt'])
                    self.dma('sp', S['ymT'][0:MW, r0:r0 + 128].rearrange("(j p) t -> p j t", p=128), ymT[:, :, :],
                             ['ymTt'], ['ymT_d'], 'ymTst', multi=True)

            HK = H * KC
            NC0 = 2 * HK * DV

            def save_state(dst, d):
                self.dma('sp', dst[:, d * HK * DV:(d + 1) * HK * DV].rearrange("p (j v) -> p j v", v=DV),
                         C32[:, d * HK:(d + 1) * HK, :], ['C32'], ['stdram'], 'stsave', multi=True)
                self.dma('sp', dst[:, NC0 + d * HK:NC0 + (d + 1) * HK], N32[:, d * HK:(d + 1) * HK], ['N32'], ['stdram'],
                         'stsave', multi=True)

            lat_f = list(range(2, NCH))
            lat_b = list(range(NCH - 1, 1, -1))
            zero_state()
            for c in (0, 1):
                chunk(c, 0, False)
            for c in (1, 0):
                chunk(c, 1, False)
            for d in range(2):
                save_state(S['cst'], d)
            zero_state()
            for c in lat_f:
                chunk(c, 0, False)
            for c in lat_b:
                chunk(c, 1, False)
            for d in range(2):
                save_state(S['send'], d)
            self.dma('sp', S['send'][:, cfg.NST:cfg.NST + 8], Bt[:, :], ['Btot'], ['stdram'], 'stsave', multi=True)
            if NR > 1:
                self.P.op('pool', lambda e: e.collective_compute("AllGather", ALU.bypass, replica_groups=[list(range(NR))],
                                                                ins=[S['send'].opt()], outs=[S['gath'].opt()]),
                          reads=['stdram'], writes=['gath'], dma_key='gath', inc=1)
            else:
                self.dma('sp', S['gath'][:, :], S['send'][:, :], ['stdram'], ['gath'], 'gath')
            with ExitStack() as p2:
                NG8 = (NR + 1) * 8
                pc = self.sb(p2, 'pcore', [128, 2 * NG8], F32)
                self.dma('sp', pc[:, :], I['percore'][:, :], [], ['pcore'], 'pcore')
                bg = self.sb(p2, 'btg', [NR, 8], F32)
                self.dma('sp', bg[:, :], S['gath'].rearrange("(r p) n -> r p n", p=128)[:, 0, cfg.NST:cfg.NST + 8],
                         ['gath'], ['btg'], 'btg')
                rhs = self.sb(p2, 'cmb_rhs', [NR, NG8], F32)
                for gsrc in range(NR + 1):
                    self.tt(rhs[:, gsrc * 8:(gsrc + 1) * 8], pc[0:NR, gsrc * 8:(gsrc + 1) * 8], bg[:, :], ALU.mult,
                            ['pcore', 'btg'], ['cmb_rhs'])
                self.mm(ps_n[:, 0:NG8], cst[0:NR, cfg.c_one:cfg.c_one + 128], rhs[:, :], True, True, ['consts', 'cmb_rhs'], ['ps_n'])
                coef = self.sb(p2, 'coef', [128, NG8], F32)
                self.act(coef[:, :], ps_n[:, 0:NG8], AF.Exp, ['ps_n'], ['coef'], scale=-1.0)
                self.tt(coef[:, :], coef[:, :], pc[:, NG8:2 * NG8], ALU.mult, ['coef', 'pcore'], ['coef'])
                self.memset(C32[:, :, :], 0.0, ['C32'])
                self.memset(N32[:, :], 0.0, ['N32'])
                ld = [self.sb(p2, f'stld{i}', [128, cfg.NST], F32) for i in range(2)]
                for gsrc in range(NR + 1):
                    s = gsrc % 2
                    src = S['cst'] if gsrc == NR else S['gath'][gsrc * 128:(gsrc + 1) * 128, 0:cfg.NST]
                    self.dma('sp', ld[s][:, :], src[:, 0:cfg.NST], ['gath', 'stdram'], [f'stld{s}'], f'stld{s}')
                    for j in range(2 * H * KC):
                        dh = j // KC
                        base = j * DV
                        cf = coef[:, gsrc * 8 + dh:gsrc * 8 + dh + 1]
                        self.stt(C32[:, j, :], ld[s][:, base:base + DV], cf, C32[:, j, :], ALU.mult, ALU.add,
                                 [f'stld{s}', 'coef', 'C32'], ['C32'])
                        self.stt(N32[:, j:j + 1], ld[s][:, NC0 + j:NC0 + j + 1], cf, N32[:, j:j + 1], ALU.mult, ALU.add,
                                 [f'stld{s}', 'coef', 'N32'], ['N32'])
                for d in range(2):
                    save_state(S['send'], d)
            self.P.fence()
            def load_state(src, d):
                jj = slice(d * HK, (d + 1) * HK)
                self.dma('sp', C32[:, jj, :], src[:, d * HK * DV:(d + 1) * HK * DV].rearrange("p (j v) -> p j v", v=DV),
                         ['stdram'], ['C32'], 'stload', multi=True)
                self.dma('sp', N32[:, jj], src[:, NC0 + d * HK:NC0 + (d + 1) * HK], ['stdram'], ['N32'], 'stload', multi=True)
                self.cp(Cb[:, jj, :], C32[:, jj, :], ['C32'], ['Cb'])
                self.cp(Nb[:, jj], N32[:, jj], ['N32'], ['Nb'])
            if full:
                zero_state()
                for c in (0, 1):
                    chunk(c, 0, True)
            load_state(S['send'], 0)
            for c in lat_f:
                chunk(c, 0, True)
            if full:
                zero_state()
                for c in (1, 0):
                    chunk(c, 1, True)
            load_state(S['send'], 1)
            for c in lat_b:
                chunk(c, 1, True)
        self.P.fence()

    def phase_gmlp(self, l):
        cfg, I, S = self.cfg, self.I, self.S
        NCH, GW, GHD, MW = cfg.NCH, cfg.GW, cfg.GHD, cfg.MW
        cst = self.cst
        with ExitStack() as ph:
            wraw = self.sb(ph, 'gws_raw', [128, 4, 128], F32)
            self.dma('sp', wraw[:, :, :], I['gmlp_ws'][l * 512:(l + 1) * 512, :].rearrange("(h t) s -> t h s", t=128),
                     [], ['gws_raw'], 'gws_raw')
            wsT = self.sb(ph, 'gwsT', [128, 4, 128], BF16)
            pst = self.ps(ph, 'g_pst', [128, 512], F32)
            for h in range(4):
                self.tr(pst[:, h * 128:(h + 1) * 128], wraw[:, h, :], cst[:, 0:128], ['gws_raw', 'consts'], ['g_pst'])
            self.P.op('dve', lambda e: e.tensor_copy(wsT[:, :, :], pst[:, :].rearrange("p (h t) -> p h t", t=128)),
                      reads=['g_pst'], writes=['gwsT'])
            braw = self.sb(ph, 'gbs_raw', [4, 128], F32)
            self.dma('sp', braw[:, :], I['gmlp_bs'][l * 4:(l + 1) * 4, :], [], ['gbs_raw'], 'gbs_raw')
            psb = self.ps(ph, 'g_psb', [128, 4], F32)
            self.tr(psb[:, 0:4], braw[:, :], cst[0:4, 0:4], ['gbs_raw', 'consts'], ['g_psb'])
            bs = self.sb(ph, 'gbs', [128, 4], F32)
            self.cp(bs[:, :], psb[:, 0:4], ['g_psb'], ['gbs'])
            gn = self.sb(ph, 'ggn', [128, GW], F32)
            self.dma('sp', gn[:, :], I['gmlp_norm_g'][l:l + 1, :].partition_broadcast(128), [], ['ggn'], 'ggn')
            u = self.sb(ph, 'g_u', [128, GW], F32)
            v = self.sb(ph, 'g_v', [128, GW], F32)
            t1 = self.sb(ph, 'g_t1', [128, GW], F32)
            vb = self.sb(ph, 'g_vb', [128, GW], BF16)
            ob = self.sb(ph, 'g_ob', [128, GW], BF16)
            oT = self.sb(ph, 'g_oT', [128, GW // 128, 128], BF16)
            ss = self.sb(ph, 'g_ss', [128, 4], F32)
            junk = self.sb(ph, 'g_junk', [128, GW], BF16)
            pss = self.ps(ph, 'g_pss', [128, GW], F32)
            ptr = self.ps(ph, 'g_ptr', [128, 1024], BF16)

            def gelu(x, key):
                self.tt(t1[:, :], x[:, :], x[:, :], ALU.mult, [key], ['g_t1'])
                self.stt(t1[:, :], t1[:, :], 0.044715, x[:, :], ALU.mult, ALU.mult, ['g_t1', key], ['g_t1'])
                self.tt(t1[:, :], t1[:, :], x[:, :], ALU.add, ['g_t1', key], ['g_t1'])
                self.act(t1[:, :], t1[:, :], AF.Sigmoid, ['g_t1'], ['g_t1'], scale=1.5957691216057308)
                self.tt(x[:, :], x[:, :], t1[:, :], ALU.mult, [key, 'g_t1'], [key])

            c0 = 0 if l == 0 else 2
            for c in range(c0, NCH):
                r0 = c * 128
                self.dma('sp', u[:, :], S['gu'][r0:r0 + 128, :], ['gu_d'], ['g_u'], 'g_u')
                self.dma('sp', v[:, :], S['gv'][r0:r0 + 128, :], ['gv_d'], ['g_v'], 'g_v')
                gelu(u, 'g_u')
                gelu(v, 'g_v')
                self.memset(ss[:, 0:1], 0.0, ['g_ss'])
                self.act(junk[:, :], v[:, :], AF.Square, ['g_v'], ['g_junk', 'g_ss'], accum_out=ss[:, 0:1])
                self.rstd(ss[:, 2:3], ss[:, 1:2], ss[:, 0:1], 1.0 / GW, 'g_ss')
                self.stt(vb[:, :], v[:, :], ss[:, 2:3], gn[:, :], ALU.mult, ALU.mult, ['g_v', 'g_ss', 'ggn'], ['g_vb'])
                for h in range(4):
                    self.mm(pss[:, h * GHD:(h + 1) * GHD], wsT[:, h, :], vb[:, h * GHD:(h + 1) * GHD], True, True,
                            ['gwsT', 'g_vb'], ['g_pss'])
                for h in range(4):
                    self.stt(ob[:, h * GHD:(h + 1) * GHD], pss[:, h * GHD:(h + 1) * GHD], bs[:, h:h + 1], u[:, h * GHD:(h + 1) * GHD],
                             ALU.add, ALU.mult, ['g_pss', 'gbs', 'g_u'], ['g_ob'])
                nt = GW // 128
                for j in range(nt):
                    self.tr(ptr[:, j * 128:(j + 1) * 128], ob[:, j * 128:(j + 1) * 128], self.idb[:, :], ['g_ob', 'idb'], ['g_ptr'])
                self.P.op('act', lambda e, nt=nt: e.activation(
                    out=oT[:, :, :], in_=ptr[:, 0:nt * 128].rearrange("p (k t) -> p k t", t=128), func=AF.Copy),
                    reads=['g_ptr'], writes=['g_oT'])
                self.dma('sp', S['ymT'][MW:MW + GW, r0:r0 + 128].rearrange("(j p) t -> p j t", p=128), oT[:, :, :],
                         ['g_oT'], ['ymT_d'], 'g_oTst', multi=True)
        self.P.fence()

    def phase_conv(self, l):
        cfg, I, S = self.cfg, self.I, self.S
        T, CW, MW, GW = cfg.T, cfg.CW, cfg.MW, cfg.GW
        cst = self.cst
        ncc = CW // 128
        with ExitStack() as ph:
            wraw = self.sb(ph, 'cw_raw', [3, CW], F32)
            self.dma('sp', wraw[:, :], I['conv_w'][l * 3:(l + 1) * 3, :], [], ['cw_raw'], 'cw_raw')
            pw = self.ps(ph, 'c_pw', [128, 4 * ncc], F32)
            for cc in range(ncc):
                self.tr(pw[:, cc * 4:cc * 4 + 3], wraw[:, cc * 128:(cc + 1) * 128], cst[0:3, 0:3], ['cw_raw', 'consts'], ['c_pw'])
            cw = self.sb(ph, 'cw', [128, 4 * ncc], F32)
            for cc in range(ncc):
                self.cp(cw[:, cc * 4:cc * 4 + 3], pw[:, cc * 4:cc * 4 + 3], ['c_pw'], ['cw'])
            bgt = self.sb(ph, 'c_b', [128, T], F32)
            cgt = self.sb(ph, 'c_c', [128, T], F32)
            xt = self.sb(ph, 'c_x', [128, T], F32)
            acc = self.sb(ph, 'c_acc', [128, T], F32)
            sh = self.sb(ph, 'c_sh', [128, T], F32)
            ob = self.sb(ph, 'c_ob', [128, T], BF16)
            mL = cst[:, cfg.c_mL:cfg.c_mL + T]
            mR = cst[:, cfg.c_mR:cfg.c_mR + T]
            for cc in range(ncc):
                rr = slice(cc * 128, (cc + 1) * 128)
                self.dma('sp', bgt[:, :], S['cb'][rr, :], ['cb_d'], ['c_b'], 'c_b')
                self.dma('sp', cgt[:, :], S['cc'][rr, :], ['cc_d'], ['c_c'], 'c_c')
                self.dma('sp', xt[:, :], S['cx'][rr, :], ['cx_d'], ['c_x'], 'c_x')
                self.tt(xt[:, :], xt[:, :], cgt[:, :], ALU.mult, ['c_x', 'c_c'], ['c_x'])
                self.ts(acc[:, :], xt[:, :], cw[:, cc * 4 + 1:cc * 4 + 2], None, ALU.mult, None, ['c_x', 'cw'], ['c_acc'])
                self.memset(sh[:, 0:1], 0.0, ['c_sh'])
                self.tt(sh[:, 1:T], xt[:, 0:T - 1], mL[:, 1:T], ALU.mult, ['c_x', 'consts'], ['c_sh'])
                self.stt(acc[:, :], sh[:, :], cw[:, cc * 4:cc * 4 + 1], acc[:, :], ALU.mult, ALU.add, ['c_sh', 'cw', 'c_acc'], ['c_acc'])
                self.memset(sh[:, T - 1:T], 0.0, ['c_sh'])
                self.tt(sh[:, 0:T - 1], xt[:, 1:T], mR[:, 0:T - 1], ALU.mult, ['c_x', 'consts'], ['c_sh'])
                self.stt(acc[:, :], sh[:, :], cw[:, cc * 4 + 2:cc * 4 + 3], acc[:, :], ALU.mult, ALU.add, ['c_sh', 'cw', 'c_acc'], ['c_acc'])
                self.tt(ob[:, :], acc[:, :], bgt[:, :], ALU.mult, ['c_acc', 'c_b'], ['c_ob'])
                self.dma('sp', S['ymT'][MW + GW + cc * 128:MW + GW + (cc + 1) * 128, :], ob[:, :], ['c_ob'], ['ymT_d'], 'c_obst', multi=True)
        self.P.fence()

    def phase_wout(self, l, cur):
        cfg, I, S = self.cfg, self.I, self.S
        D, T, KD, NCH = cfg.D, cfg.T, cfg.KD, cfg.NCH
        W = I['w_out'][l * D:(l + 1) * D, :]
        with ExitStack() as ph:
            yT = self.sb(ph, 'yT', [128, KD, T], BF16)
            src = S['ymT'].rearrange("(k p) t -> p k t", p=128)
            for k0 in range(0, KD, 8):
                k1 = min(KD, k0 + 8)
                self.dma('sp', yT[:, k0:k1, :], src[:, k0:k1, :], ['ymT_d'], ['yT'], 'yT', multi=True)
            m2 = [self.load_mod_bc(ph, f'm2_{w}', l, 2, w) for w in range(2)]
            wt = [self.sb(ph, f'wow{s}', [128, KD, 512], BF16) for s in range(2)]
            pacc = [self.ps(ph, f'wo_ps{s}', [128, 512], F32) for s in range(2)]
            xs = [self.sb(ph, f'wo_x{s}', [128, 512], F32) for s in range(2)]
            tm = [self.sb(ph, f'wo_tmp{s}', [128, 512], F32) for s in range(2)]
            c0 = 0 if l == 0 else 2
            ctr = 0
            for cb in range(0, D, 512):
                s = (cb // 512) % 2
                self.load_w(wt, s, 'wow', W, KD, cb, 512)
                for tb in range(c0, NCH):
                    p = ctr % 2
                    ctr += 1
                    w = 1 if tb < 2 else 0
                    self.dma('sp', xs[p][:, :], cur[tb * 128:(tb + 1) * 128, cb:cb + 512], ['xcur_d'], [f'wo_x{p}'], f'wo_x{p}')
                    for k in range(KD):
                        self.mm(pacc[p][:, :], yT[:, k, tb * 128:(tb + 1) * 128], wt[s][:, k, :], k == 0, k == KD - 1,
                                ['yT', f'wow{s}'], [f'wo_ps{p}'])
                    self.tt(tm[p][:, :], pacc[p][:, :], m2[w][:, cb:cb + 512], ALU.mult, [f'wo_ps{p}', f'm2_{w}'], [f'wo_tmp{p}'])
                    self.tt(xs[p][:, :], xs[p][:, :], tm[p][:, :], ALU.add, [f'wo_x{p}', f'wo_tmp{p}'], [f'wo_x{p}'])
                    self.dma('sp', S['xmix'][tb * 128:(tb + 1) * 128, cb:cb + 512], xs[p][:, :], [f'wo_x{p}'], ['xmix_d'],
                             f'wo_st{p}', multi=True)
        self.P.fence()

    def phase_moe(self, l, last):
        cfg, I, S = self.cfg, self.I, self.S
        D, T, KD, NCH, DFF, FB = cfg.D, cfg.T, cfg.KD, cfg.NCH, cfg.DFF, cfg.FB
        cst = self.cst
        c0 = 0 if l == 0 else 2
        if last:
            c0 = 2
        with ExitStack() as ph:
            comb = self.sb(ph, 'comb', [128, NCH, 16], F32)
            self.memset(comb[:, :, :], 0.0, ['comb'])
            with ExitStack() as p2:
                A = [self.load_mod_bc(p2, f'n2A{w}', l, 4, w, True, I['norm2_g'][l:l + 1, :]) for w in range(2)]
                Bt = [self.load_mod_bc(p2, f'n2B{w}', l, 3, w) for w in range(2)]
                xt = [self.sb(p2, f'n2x{s}', [128, D], F32) for s in range(2)]
                sq = dict(ss=self.sb(p2, 'n2ss', [128, 4], F32), junk=self.sb(p2, 'n2junk', [128, D], BF16),
                          pt=[self.ps(p2, f'n2pt{i}', [128, 512], F32) for i in range(2)])
                hb = self.sb(p2, 'h2blk', [128, KD, 128], BF16)
                k32 = self.sb(p2, 'h2k32', [128, KD, 128], F32)
                wr = self.sb(p2, 'wr', [128, KD, 16], F32)
                self.dma('sp', wr[:, :, :], I['w_router'].rearrange("(k p) e -> p k e", p=128), [], ['wr'], 'wr')
                br = self.sb(p2, 'br', [128, 16], F32)
                self.dma('sp', br[:, :], I['b_router'][0:1, :].partition_broadcast(128), [], ['br'], 'br')
                pr = self.ps(p2, 'n2pr', [128, 16], F32)
                sc = self.sb(p2, 'r_sc', [128, 16], F32)
                sel = self.sb(p2, 'r_sel', [128, 16], F32)
                t16 = self.sb(p2, 'r_t16', [128, 16], F32)
                m1 = self.sb(p2, 'r_m1', [128, 4], F32)
                m2 = self.sb(p2, 'r_m2', [128, 4], F32)
                gs = self.sb(p2, 'r_gs', [128, 4], F32)
                g1 = self.sb(p2, 'r_g1', [128, 4], F32)
                gmx = self.sb(p2, 'r_gmx', [128, 4], F32)
                for tb in range(c0, NCH):
                    w = 1 if tb < 2 else 0
                    self.norm_mod_T(p2, 'n2', S['xmix'], tb, A[w], Bt[w], f'n2A{w}', f'n2B{w}', hb, 'h2blk', 0, xt, sq, keep32=k32)
                    self.dma('sp', S['h2T'][tb * 128:(tb + 1) * 128, :].rearrange("p (k t) -> p k t", t=128), hb[:, :, :],
                             ['h2blk'], ['h2T_d'], 'h2Tst', multi=True)
                    if cfg.stop == f'n2{l}':
                        continue
                    for k in range(KD):
                        self.mm(pr[:, 0:16], k32[:, k, :], wr[:, k, :], k == 0, k == KD - 1, ['n2k32', 'wr'], ['n2pr'])
                    self.act(sc[:, :], pr[:, 0:16], AF.Sigmoid, ['n2pr'], ['r_sc'])
                    self.tt(sel[:, :], sc[:, :], br[:, :], ALU.add, ['r_sc', 'br'], ['r_sel'])
                    sel3 = sel[:, :].rearrange("p (g e) -> p g e", e=4)
                    t3 = t16[:, :].rearrange("p (g e) -> p g e", e=4)
                    def red(out, in3, key_in, key_out):
                        self.P.op('dve', lambda e: e.tensor_reduce(out, in3, mybir.AxisListType.X, ALU.max),
                                  reads=[key_in], writes=[key_out])
                    red(m1[:, :], sel3, 'r_sel', 'r_m1')
                    for gi in range(4):
                        self.ts(t16[:, gi * 4:(gi + 1) * 4], sel[:, gi * 4:(gi + 1) * 4], m1[:, gi:gi + 1], -1e30, ALU.is_ge, ALU.mult,
                                ['r_sel', 'r_m1'], ['r_t16'])
                    self.tt(t16[:, :], t16[:, :], sel[:, :], ALU.add, ['r_t16', 'r_sel'], ['r_t16'])
                    red(m2[:, :], t3, 'r_t16', 'r_m2')
                    self.tt(gs[:, :], m1[:, :], m2[:, :], ALU.add, ['r_m1', 'r_m2'], ['r_gs'])
                    self.P.op('dve', lambda e: e.tensor_reduce(gmx[:, 0:1], gs[:, :], mybir.AxisListType.X, ALU.max),
                              reads=['r_gs'], writes=['r_gmx'])
                    self.ts(g1[:, 0:4], gs[:, :], gmx[:, 0:1], None, ALU.is_ge, None, ['r_gs', 'r_gmx'], ['r_g1'])
                    for gi in range(4):
                        self.ts(t16[:, gi * 4:(gi + 1) * 4], sel[:, gi * 4:(gi + 1) * 4], m2[:, gi:gi + 1], g1[:, gi:gi + 1],
                                ALU.is_ge, ALU.mult, ['r_sel', 'r_m2', 'r_g1'], ['r_t16'])
                    self.tt(t16[:, :], t16[:, :], sc[:, :], ALU.mult, ['r_t16', 'r_sc'], ['r_t16'])
                    self.P.op('dve', lambda e: e.tensor_reduce(gmx[:, 1:2], t16[:, :], mybir.AxisListType.X, ALU.add),
                              reads=['r_t16'], writes=['r_gmx'])
                    self.P.op('dve', lambda e: e.reciprocal(gmx[:, 2:3], gmx[:, 1:2]), reads=['r_gmx'], writes=['r_gmx'])
                    self.ts(comb[:, tb, :], t16[:, :], gmx[:, 2:3], None, ALU.mult, None, ['r_t16', 'r_gmx'], ['comb'])
            self.P.fence()
            if 'comb_d' in cfg.dbg:
                cd = self.dram('comb_d', [128, NCH * 16], F32)
                self.dma('sp', cd[:, :], comb[:, :, :].rearrange("p c e -> p (c e)"), ['comb'], ['comb_dd'], 'comb_dd', final=True)
            if cfg.stop in (f'rt{l}', f'n2{l}'):
                self.P.fence()
                return
            tbs = list(range(c0, NCH))
            NTB = len(tbs)
            NT = NTB * 128
            pieces = [(t0, min(512, NT - t0)) for t0 in range(0, NT, 512)]
            aTd = S['aT']
            with ExitStack() as pa:
                h2 = self.sb(pa, 'h2g', [128, KD, NT], BF16)
                for i, tb in enumerate(tbs):
                    self.dma('sp', h2[:, :, i * 128:(i + 1) * 128], S['h2T'][tb * 128:(tb + 1) * 128, :].rearrange("p (k t) -> p k t", t=128),
                             ['h2T_d'], ['h2g'], 'h2g', multi=True)
                WC = 256 if DFF % 256 == 0 else 128
                NF = WC // 128
                wg = [self.sb(pa, f'wg{s}', [128, KD, WC], BF16) for s in range(2)]
                wu = [self.sb(pa, f'wu{s}', [128, KD, WC], BF16) for s in range(2)]
                sg = [self.sb(pa, f'sgt{s}', [128, 512], F32) for s in range(2)]
                ao = [self.sb(pa, f'aTo{s}', [128, NT], BF16) for s in range(2)]
                pg = [self.ps(pa, f'moe_pg{i}', [128, 512], F32) for i in range(len(pieces))]
                pu = [self.ps(pa, f'moe_pu{i}', [128, 512], F32) for i in range(len(pieces))]
                fctr = 0
                octr = 0
                sctr = 0
                for ex in range(16):
                    Wg = I['w_gate_e'][(l * 16 + ex) * D:(l * 16 + ex + 1) * D, :]
                    Wu = I['w_up_e'][(l * 16 + ex) * D:(l * 16 + ex + 1) * D, :]
                    for f0 in range(0, FB, NF):
                        s = fctr % 2
                        fctr += 1
                        self.load_w(wg, s, 'wg', Wg, KD, f0 * 128, WC)
                        self.load_w(wu, s, 'wu', Wu, KD, f0 * 128, WC)
                        for fi in range(NF):
                            f = f0 + fi
                            for pi, (t0, tn) in enumerate(pieces):
                                for k in range(KD):
                                    self.mm(pg[pi][:, 0:tn], wg[s][:, k, fi * 128:(fi + 1) * 128], h2[:, k, t0:t0 + tn], k == 0, k == KD - 1,
                                            ['h2g', f'wg{s}'], [f'moe_pg{pi}'])
                                for k in range(KD):
                                    self.mm(pu[pi][:, 0:tn], wu[s][:, k, fi * 128:(fi + 1) * 128], h2[:, k, t0:t0 + tn], k == 0, k == KD - 1,
                                            ['h2g', f'wu{s}'], [f'moe_pu{pi}'])
                            o = octr % 2
                            octr += 1
                            for pi, (t0, tn) in enumerate(pieces):
                                q = sctr % 2
                                sctr += 1
                                self.act(sg[q][:, 0:tn], pg[pi][:, 0:tn], AF.Silu, [f'moe_pg{pi}'], [f'sgt{q}'])
                                self.tt(ao[o][:, t0:t0 + tn], sg[q][:, 0:tn], pu[pi][:, 0:tn], ALU.mult, [f'sgt{q}', f'moe_pu{pi}'], [f'aTo{o}'])
                            r0 = (ex * FB + f) * 128
                            self.dma('sp', aTd[r0:r0 + 128, 0:NT], ao[o][:, :], [f'aTo{o}'], ['aT_d'], f'aTst{o}', multi=True)
            self.P.fence()
            with ExitStack() as pb:
                HC = D // 2 if D >= 1024 else D
                NH = D // HC
                acc = self.sb(pb, 'macc', [128, NTB, HC], F32)
                aT = [self.sb(pb, f'aT{s}', [128, FB, NT], BF16) for s in range(2)]
                wd = [self.sb(pb, f'wd{s}', [128, FB, 512], BF16) for s in range(2)]
                pd = [self.ps(pb, f'moe_pd{s}', [128, 512], F32) for s in range(4)]
                stg = [self.sb(pb, f'mo_stg{s}', [128, 512], F32) for s in range(2)]
                m5t = [self.sb(pb, f'mo_m5{s}', [128, 512], F32) for s in range(2)]
                ssq = self.sb(pb, 'mo_ssq', [128, NCH], F32)
                ss = self.sb(pb, 'mo_ss', [128, 4], F32)
                junk = self.sb(pb, 'mo_junk', [128, HC], BF16) if last else None
                if last:
                    self.memset(ssq[:, :], 0.0, ['mo_ssq'])
                dctr = 0
                pctr = 0
                sctr = 0
                actr = 0
                for hh in range(NH):
                    self.memset(acc[:, :, :], 0.0, ['macc'], eng='pool')
                    for ex in range(16):
                        Wd = I['w_down_e'][(l * 16 + ex) * DFF:(l * 16 + ex + 1) * DFF, :]
                        sa = actr % 2
                        actr += 1
                        self.dma('sp', aT[sa][:, :, :], aTd[ex * FB * 128:(ex + 1) * FB * 128, 0:NT].rearrange("(f p) t -> p f t", p=128),
                                 ['aT_d'], [f'aT{sa}'], f'aT{sa}')
                        for cb in range(0, HC, 512):
                            s = dctr % 2
                            dctr += 1
                            self.load_w(wd, s, 'wd', Wd, FB, hh * HC + cb, 512)
                            for i, tb in enumerate(tbs):
                                p = pctr % 4
                                pctr += 1
                                for f in range(FB):
                                    self.mm(pd[p][:, 0:512], aT[sa][:, f, i * 128:(i + 1) * 128], wd[s][:, f, :], f == 0, f == FB - 1,
                                            [f'aT{sa}', f'wd{s}'], [f'moe_pd{p}'])
                                self.stt(acc[:, i, cb:cb + 512], pd[p][:, 0:512], comb[:, tb, ex:ex + 1], acc[:, i, cb:cb + 512],
                                         ALU.mult, ALU.add, [f'moe_pd{p}', 'comb', 'macc'], ['macc'])
                    for i, tb in enumerate(tbs):
                        w = 1 if tb < 2 else 0
                        for cb in range(0, HC, 512):
                            q = sctr % 2
                            sctr += 1
                            gc = hh * HC + cb
                            self.dma('sp', stg[q][:, :], S['xmix'][tb * 128:(tb + 1) * 128, gc:gc + 512], ['xmix_d'], [f'mo_stg{q}'], f'mo_stg{q}')
                            self.dma('sp', m5t[q][:, :], S['mod'][w:w + 1, 5 * D + gc:5 * D + gc + 512].partition_broadcast(128),
                                     ['mod_d'], [f'mo_m5{q}'], f'mo_m5{q}')
                            self.tt(acc[:, i, cb:cb + 512], acc[:, i, cb:cb + 512], m5t[q][:, :], ALU.mult, ['macc', f'mo_m5{q}'], ['macc'])
                            self.tt(acc[:, i, cb:cb + 512], acc[:, i, cb:cb + 512], stg[q][:, :], ALU.add, ['macc', f'mo_stg{q}'], ['macc'])
                        if last:
                            self.memset(ss[:, 0:1], 0.0, ['mo_ss'])
                            self.act(junk[:, :], acc[:, i, :], AF.Square, ['macc'], ['mo_junk', 'mo_ss'], accum_out=ss[:, 0:1])
                            self.tt(ssq[:, tb:tb + 1], ssq[:, tb:tb + 1], ss[:, 0:1], ALU.add, ['mo_ssq', 'mo_ss'], ['mo_ssq'])
                        self.dma('sp', S['xa'][tb * 128:(tb + 1) * 128, hh * HC:(hh + 1) * HC], acc[:, i, :], ['macc'], ['xa_d'], 'mo_xst', multi=True)
                if last:
                    fg = self.sb(pb, 'fgb', [128, 512], F32)
                    rs = self.sb(pb, 'mo_rs', [128, NCH], F32)
                    rt = self.sb(pb, 'mo_rt', [128, NCH], F32)
                    self.rstd(rs[:, :], rt[:, :], ssq[:, :], 1.0 / D, 'mo_ssq')
                    for cb in range(0, D, 512):
                        self.dma('sp', fg[:, :], I['final_g'][0:1, cb:cb + 512].partition_broadcast(128), [], ['fgb'], 'fgb')
                        for tb in tbs:
                            q = sctr % 2
                            sctr += 1
                            self.dma('sp', stg[q][:, :], S['xa'][tb * 128:(tb + 1) * 128, cb:cb + 512], ['xa_d'], [f'mo_stg{q}'], f'mo_stg{q}')
                            self.stt(stg[q][:, :], stg[q][:, :], rs[:, tb:tb + 1], fg[:, :], ALU.mult, ALU.mult,
                                     [f'mo_stg{q}', 'mo_ssq', 'fgb'], [f'mo_stg{q}'])
                            self.dma('sp', self.y[(tb - 2) * 128:(tb - 1) * 128, cb:cb + 512], stg[q][:, :], [f'mo_stg{q}'], ['y'],
                                     f'yout{q}', final=True, multi=True)
        self.P.fence()


def make_in_maps(cfg, inputs):
    D = cfg.D
    f = lambda a: np.ascontiguousarray(np.asarray(a, dtype=np.float32))
    x, c, ctx, c_ctx = f(inputs['x']), f(inputs['c']), f(inputs['ctx']), f(inputs['c_ctx'])
    DEPTH = cfg.DEPTH
    shared = {
        'consts': consts_array(cfg),
        'w_mod': f(inputs['w_mod']).reshape(DEPTH * D, 6 * D),
        'b_mod': f(inputs['b_mod']).reshape(DEPTH, 6 * D),
        'norm1_g': f(inputs['norm1_g']), 'norm2_g': f(inputs['norm2_g']),
        'w_in': f(inputs['w_in']).reshape(DEPTH * D, cfg.INC),
        'b_gates': f(inputs['b_gates']).reshape(DEPTH, 16),
        'mlstm_norm_g': f(inputs['mlstm_norm_g']),
        'gmlp_ws': f(inputs['gmlp_ws']).reshape(DEPTH * 4 * 128, 128),
        'gmlp_bs': f(inputs['gmlp_bs']).reshape(DEPTH * 4, 128),
        'gmlp_norm_g': f(inputs['gmlp_norm_g']),
        'conv_w': f(inputs['conv_w']).reshape(DEPTH * 3, cfg.CW),
        'w_out': f(inputs['w_out']).reshape(DEPTH * D, D),
        'w_router': f(inputs['w_router']), 'b_router': f(inputs['b_router']).reshape(1, 16),
        'w_gate_e': f(inputs['w_gate_e']).reshape(DEPTH * 16 * D, cfg.DFF),
        'w_up_e': f(inputs['w_up_e']).reshape(DEPTH * 16 * D, cfg.DFF),
        'w_down_e': f(inputs['w_down_e']).reshape(DEPTH * 16 * cfg.DFF, D),
        'final_g': f(inputs['final_g']).reshape(1, D),
    }
    maps = []
    for core in range(cfg.NCORES):
        b, r = divmod(core, cfg.SEG)
        m = dict(shared)
        m['xin'] = np.ascontiguousarray(np.concatenate([ctx[b], x[b, r * cfg.TL:(r + 1) * cfg.TL]], axis=0))
        m['cvec'] = np.ascontiguousarray(np.stack([c[b], c_ctx], axis=0))
        m['percore'] = percore_array(cfg, core)
        maps.append(m)
    return maps


def run_cfg(cfg, inputs, trace=False):
    nc = Builder(cfg).build()
    maps = make_in_maps(cfg, inputs)
    res = run_bass_kernel_spmd(nc, maps, core_ids=list(range(cfg.NCORES)), trace=trace)
    return res


def kernel(**inputs):
    cfg = Cfg()
    res = run_cfg(cfg, inputs)
    out = np.zeros((cfg.BATCH, cfg.SEG * cfg.TL, cfg.D), np.float32)
    for core in range(cfg.NCORES):
        b, r = divmod(core, cfg.SEG)
        out[b, r * cfg.TL:(r + 1) * cfg.TL, :] = np.asarray(res.results[core]['y'], dtype=np.float32)
    return out
```
